# Optimizing a Trainium2 kernel written in Bass

```python
import jax
import jax.numpy as jnp
from jax import lax
import numpy as np

D_MODEL = 1024
BATCH = 4
SEQ = 8192
DEPTH = 2

MLSTM_HEADS = 4
MLSTM_DQK = 64
MLSTM_DV = 128
MLSTM_CHUNK = 64
CONV_WIDTH = 4
MOBA_HEADS = 4
MOBA_DH = 64
MOBA_BLOCK = 256
MOBA_TOPK = 3
MOBA_Q_BLOCK = 64
ROPE_THETA = 500000.0
PARTIAL_ROPE_DIM = MOBA_DH // 4
MLA_HEADS = 4
MLA_NOPE = 64
MLA_ROPE = 32
MLA_DV = 64
MLA_Q_LORA = 384
MLA_KV_LORA = 256
MLA_ROPE_THETA = 10000.0
ATTN_Q_BLOCK = 128
D_FF = 4 * D_MODEL
NORM_EPS = 1e-6

MLSTM_WIDTH = MLSTM_HEADS * MLSTM_DV
MOBA_WIDTH = MOBA_HEADS * MOBA_DH
MLA_WIDTH = MLA_HEADS * MLA_DV
MIX_WIDTH = MLSTM_WIDTH + MOBA_WIDTH + MLA_WIDTH
MLA_QK_DIM = MLA_NOPE + MLA_ROPE
IN_SPLITS = (
    2 * MLSTM_HEADS * MLSTM_DQK,
    MLSTM_WIDTH,
    MLSTM_WIDTH,
    MLSTM_HEADS,
    MLSTM_HEADS,
    3 * MOBA_WIDTH,
    MLA_Q_LORA,
    MLA_KV_LORA,
    MLA_ROPE,
)
D_IN = sum(IN_SPLITS)

kernel_name = 'hybrid_mlstm_moba_mla_block'


def rms_norm(x, g):
    xf = x.astype(jnp.float32)
    y = xf * lax.rsqrt(jnp.mean(xf * xf, axis=-1, keepdims=True) + NORM_EPS)
    return (y * g.astype(jnp.float32)).astype(x.dtype)


def rope_tables(positions, dim, theta):
    inv_freq = jnp.power(jnp.float32(theta), -jnp.arange(0, dim, 2, dtype=jnp.float32) / dim)
    ang = positions.astype(jnp.float32)[..., None] * inv_freq
    return jnp.cos(ang), jnp.sin(ang)


def rotate(x, cos, sin):
    half = x.shape[-1] // 2
    x1 = x[..., :half].astype(jnp.float32)
    x2 = x[..., half:].astype(jnp.float32)
    c = cos[:, :, None, :]
    s = sin[:, :, None, :]
    return jnp.concatenate([x1 * c - x2 * s, x2 * c + x1 * s], axis=-1).astype(x.dtype)


def causal_depthwise_conv(x, w, b):
    C = x.shape[-1]
    y = lax.conv_general_dilated(
        x, w[:, None, :].astype(x.dtype), window_strides=(1,),
        padding=[(CONV_WIDTH - 1, 0)], dimension_numbers=('NWC', 'WIO', 'NWC'),
        feature_group_count=C)
    return y + b.astype(x.dtype)


def mlstm_chunkwise(q, k, v, i_pre, f_pre):
    B, S, H, DK = q.shape
    DV = v.shape[-1]
    L = MLSTM_CHUNK
    NC = S // L
    f32 = jnp.float32
    q = q.astype(f32) * (DK ** -0.5)
    k = k.astype(f32)
    v = v.astype(f32)
    log_f = jax.nn.log_sigmoid(f_pre.astype(f32))
    log_i = i_pre.astype(f32)

    def chunks(t):
        return t.reshape((B, NC, L) + t.shape[2:]).swapaxes(0, 1)

    causal = jnp.tril(jnp.ones((L, L), dtype=bool))

    def step(carry, inp):
        C, n, m = carry
        qj, kj, vj, lfj, lij = inp
        bt = jnp.cumsum(lfj, axis=1).transpose(0, 2, 1)
        it = lij.transpose(0, 2, 1)
        log_d = bt[:, :, :, None] - bt[:, :, None, :] + it[:, :, None, :]
        log_d = jnp.where(causal, log_d, -jnp.inf)
        log_inter = bt + m[:, :, None]
        m_t = jnp.maximum(log_inter, jnp.max(log_d, axis=-1))
        d = jnp.exp(log_d - m_t[..., None])
        inter = jnp.exp(log_inter - m_t)
        s = jnp.einsum('blhd,bshd->bhls', qj, kj) * d
        num = (jnp.einsum('bhls,bshv->bhlv', s, vj)
               + inter[..., None] * jnp.einsum('blhd,bhvd->bhlv', qj, C))
        den = jnp.sum(s, axis=-1) + inter * jnp.einsum('blhd,bhd->bhl', qj, n)
        h = num / jnp.maximum(jnp.abs(den), jnp.exp(-m_t))[..., None]
        b_last = bt[:, :, -1]
        log_w = b_last[:, :, None] - bt + it
        m_new = jnp.maximum(b_last + m, jnp.max(log_w, axis=-1))
        w = jnp.exp(log_w - m_new[..., None])
        decay = jnp.exp(b_last + m - m_new)
        C_new = decay[..., None, None] * C + jnp.einsum('bhs,bshv,bshd->bhvd', w, vj, kj)
        n_new = decay[..., None] * n + jnp.einsum('bhs,bshd->bhd', w, kj)
        return (C_new, n_new, m_new), h.transpose(0, 2, 1, 3)

    init = (jnp.zeros((B, H, DV, DK), f32), jnp.zeros((B, H, DK), f32), jnp.zeros((B, H), f32))
    _, h = lax.scan(step, init, (chunks(q), chunks(k), chunks(v), chunks(log_f), chunks(log_i)))
    return h.swapaxes(0, 1).reshape(B, S, H, DV)


def moba_attention(q, k, v):
    B, S, H, D = q.shape
    BS = MOBA_BLOCK
    QB = MOBA_Q_BLOCK
    NB = -(-S // BS)
    S_pad = NB * BS
    K = min(MOBA_TOPK, NB)
    scale = D ** -0.5
    qh = q.transpose(0, 2, 1, 3)
    pad = ((0, 0), (0, 0), (0, S_pad - S), (0, 0))
    kb = jnp.pad(k.transpose(0, 2, 1, 3), pad).reshape(B, H, NB, BS, D)
    vb = jnp.pad(v.transpose(0, 2, 1, 3), pad).reshape(B, H, NB, BS, D)
    k_mean = jnp.mean(kb.astype(jnp.float32), axis=3)
    b_idx = jnp.arange(B)[:, None, None, None]
    h_idx = jnp.arange(H)[None, :, None, None]

    def one_block(qi):
        start = qi * QB
        q_blk = lax.dynamic_slice_in_dim(qh, start, QB, axis=2)
        own = start // BS
        gate = jnp.einsum('bhqd,bhnd->bhqn', q_blk.astype(jnp.float32), k_mean)
        gate = jnp.where(jnp.arange(NB) < own, gate, -jnp.inf)
        _, sel = lax.top_k(gate, K)
        sel_valid = jnp.arange(K) < own
        k_sel = kb[b_idx, h_idx, sel]
        v_sel = vb[b_idx, h_idx, sel]
        s_sel = jnp.einsum('bhqd,bhqkjd->bhqkj', q_blk, k_sel).astype(jnp.float32) * scale
        s_sel = jnp.where(sel_valid[:, None], s_sel, -jnp.inf)
        k_own = lax.dynamic_index_in_dim(kb, own, axis=2, keepdims=False)
        v_own = lax.dynamic_index_in_dim(vb, own, axis=2, keepdims=False)
        s_own = jnp.einsum('bhqd,bhjd->bhqj', q_blk, k_own).astype(jnp.float32) * scale
        q_pos = start + jnp.arange(QB)
        k_pos = own * BS + jnp.arange(BS)
        s_own = jnp.where(k_pos[None, :] <= q_pos[:, None], s_own, -jnp.inf)
        s = jnp.concatenate([s_sel.reshape(B, H, QB, K * BS), s_own], axis=-1)
        p = jax.nn.softmax(s, axis=-1).astype(v.dtype)
        p_sel = p[..., :K * BS].reshape(B, H, QB, K, BS)
        p_own = p[..., K * BS:]
        return (jnp.einsum('bhqkj,bhqkjd->bhqd', p_sel, v_sel)
                + jnp.einsum('bhqj,bhjd->bhqd', p_own, v_own))

    out = lax.map(one_block, jnp.arange(S // QB))
    return out.transpose(1, 0, 3, 2, 4).reshape(B, S, H, D)


def causal_block_attention(q, k, v):
    B, S, H, Dk = q.shape
    Dv = v.shape[-1]
    QB = ATTN_Q_BLOCK
    scale = Dk ** -0.5
    qh = q.transpose(0, 2, 1, 3)
    kh = k.transpose(0, 2, 1, 3)
    vh = v.transpose(0, 2, 1, 3)
    k_pos = jnp.arange(S)

    def one_block(qi):
        start = qi * QB
        q_blk = lax.dynamic_slice_in_dim(qh, start, QB, axis=2)
        s = jnp.einsum('bhqd,bhkd->bhqk', q_blk, kh).astype(jnp.float32) * scale
        q_pos = start + jnp.arange(QB)
        s = jnp.where(k_pos[None, :] <= q_pos[:, None], s, -jnp.inf)
        p = jax.nn.softmax(s, axis=-1).astype(vh.dtype)
        return jnp.einsum('bhqk,bhkd->bhqd', p, vh)

    out = lax.map(one_block, jnp.arange(S // QB))
    return out.transpose(1, 0, 3, 2, 4).reshape(B, S, H, Dv)


def hybrid_layer(x, cos_p, sin_p, cos_d, sin_d, w_in, conv_w, conv_b, b_igate, b_fgate,
                 g_mix_norm, g_mlstm_out, g_moba_q, g_moba_k, g_moba_out, g_cq, g_ckv,
                 w_uq, w_ukv, g_mla_q, g_mla_k, g_mla_out, w_out, g_mlp_norm, w_up, w_down):
    B, S, _ = x.shape
    h = rms_norm(x, g_mix_norm)
    proj = h @ w_in
    offsets = [int(o) for o in np.cumsum(IN_SPLITS)[:-1]]
    m_qk, m_v, m_o, m_i, m_f, moba_qkv, cq, ckv, kpe = jnp.split(proj, offsets, axis=-1)

    qk = jax.nn.silu(causal_depthwise_conv(m_qk, conv_w, conv_b))
    mq, mk = jnp.split(qk, 2, axis=-1)
    mq = mq.reshape(B, S, MLSTM_HEADS, MLSTM_DQK)
    mk = mk.reshape(B, S, MLSTM_HEADS, MLSTM_DQK)
    mv = m_v.reshape(B, S, MLSTM_HEADS, MLSTM_DV)
    hm = mlstm_chunkwise(mq, mk, mv, m_i + b_igate, m_f + b_fgate).astype(x.dtype)
    hm = rms_norm(hm, g_mlstm_out) * jax.nn.sigmoid(m_o).reshape(B, S, MLSTM_HEADS, MLSTM_DV)

    aq, ak, av = jnp.split(moba_qkv.reshape(B, S, 3 * MOBA_HEADS, MOBA_DH), 3, axis=2)
    aq = rms_norm(aq, g_moba_q)
    ak = rms_norm(ak, g_moba_k)
    aq = jnp.concatenate([rotate(aq[..., :PARTIAL_ROPE_DIM], cos_p, sin_p), aq[..., PARTIAL_ROPE_DIM:]], axis=-1)
    ak = jnp.concatenate([rotate(ak[..., :PARTIAL_ROPE_DIM], cos_p, sin_p), ak[..., PARTIAL_ROPE_DIM:]], axis=-1)
    ha = rms_norm(moba_attention(aq, ak, av), g_moba_out)

    lq = (rms_norm(cq, g_cq) @ w_uq).reshape(B, S, MLA_HEADS, MLA_QK_DIM)
    kv = (rms_norm(ckv, g_ckv) @ w_ukv).reshape(B, S, MLA_HEADS, MLA_NOPE + MLA_DV)
    lk = jnp.concatenate(
        [kv[..., :MLA_NOPE], jnp.broadcast_to(kpe[:, :, None, :], (B, S, MLA_HEADS, MLA_ROPE))], axis=-1)
    lv = kv[..., MLA_NOPE:]
    lq = rms_norm(lq, g_mla_q)
    lk = rms_norm(lk, g_mla_k)
    lq = jnp.concatenate([lq[..., :MLA_NOPE], rotate(lq[..., MLA_NOPE:], cos_d, sin_d)], axis=-1)
    lk = jnp.concatenate([lk[..., :MLA_NOPE], rotate(lk[..., MLA_NOPE:], cos_d, sin_d)], axis=-1)
    hl = rms_norm(causal_block_attention(lq, lk, lv), g_mla_out)

    mix = jnp.concatenate([hm.reshape(B, S, MLSTM_WIDTH), ha.reshape(B, S, MOBA_WIDTH),
                           hl.reshape(B, S, MLA_WIDTH)], axis=-1)
    x = x + mix @ w_out

    h2 = rms_norm(x, g_mlp_norm)
    x = x + jnp.square(jax.nn.relu(h2 @ w_up)) @ w_down
    return x


def setup_inputs(seed: int = 0) -> dict:
    key = jax.random.key(seed)
    ks = jax.random.split(key, 24)
    f32 = jnp.float32

    def nrm(k, shape, scale):
        return jax.random.normal(k, shape, f32) * scale

    def gain(k, shape):
        return 1.0 + 0.02 * jax.random.normal(k, shape, f32)

    Ld = DEPTH
    x = nrm(ks[0], (BATCH, SEQ, D_MODEL), 1.0)
    offset = jax.random.randint(ks[1], (BATCH, 1), 0, 4096, dtype=jnp.int32)
    positions = offset + jnp.arange(SEQ, dtype=jnp.int32)[None, :]
    return {
        'x': x,
        'positions': positions,
        'w_in': nrm(ks[2], (Ld, D_MODEL, D_IN), D_MODEL ** -0.5),
        'conv_w': nrm(ks[3], (Ld, CONV_WIDTH, 2 * MLSTM_HEADS * MLSTM_DQK), 0.5),
        'conv_b': nrm(ks[4], (Ld, 2 * MLSTM_HEADS * MLSTM_DQK), 0.01),
        'b_igate': nrm(ks[5], (Ld, MLSTM_HEADS), 0.1),
        'b_fgate': 3.0 + nrm(ks[6], (Ld, MLSTM_HEADS), 0.5),
        'g_mix_norm': gain(ks[7], (Ld, D_MODEL)),
        'g_mlstm_out': gain(ks[8], (Ld, MLSTM_HEADS, MLSTM_DV)),
        'g_moba_q': gain(ks[9], (Ld, MOBA_DH)),
        'g_moba_k': gain(ks[10], (Ld, MOBA_DH)),
        'g_moba_out': gain(ks[11], (Ld, MOBA_HEADS, MOBA_DH)),
        'g_cq': gain(ks[12], (Ld, MLA_Q_LORA)),
        'g_ckv': gain(ks[13], (Ld, MLA_KV_LORA)),
        'w_uq': nrm(ks[14], (Ld, MLA_Q_LORA, MLA_HEADS * MLA_QK_DIM), MLA_Q_LORA ** -0.5),
        'w_ukv': nrm(ks[15], (Ld, MLA_KV_LORA, MLA_HEADS * (MLA_NOPE + MLA_DV)), MLA_KV_LORA ** -0.5),
        'g_mla_q': gain(ks[16], (Ld, MLA_QK_DIM)),
        'g_mla_k': gain(ks[17], (Ld, MLA_QK_DIM)),
        'g_mla_out': gain(ks[18], (Ld, MLA_HEADS, MLA_DV)),
        'w_out': nrm(ks[19], (Ld, MIX_WIDTH, D_MODEL), 0.5 * MIX_WIDTH ** -0.5),
        'g_mlp_norm': gain(ks[20], (Ld, D_MODEL)),
        'w_up': nrm(ks[21], (Ld, D_MODEL, D_FF), D_MODEL ** -0.5),
        'w_down': nrm(ks[22], (Ld, D_FF, D_MODEL), 0.5 * D_FF ** -0.5),
    }


def reference(x, positions, w_in, conv_w, conv_b, b_igate, b_fgate, g_mix_norm, g_mlstm_out,
              g_moba_q, g_moba_k, g_moba_out, g_cq, g_ckv, w_uq, w_ukv, g_mla_q, g_mla_k,
              g_mla_out, w_out, g_mlp_norm, w_up, w_down):
    cos_p, sin_p = rope_tables(positions, PARTIAL_ROPE_DIM, ROPE_THETA)
    cos_d, sin_d = rope_tables(positions, MLA_ROPE, MLA_ROPE_THETA)
    for l in range(DEPTH):
        x = hybrid_layer(
            x, cos_p, sin_p, cos_d, sin_d, w_in[l], conv_w[l], conv_b[l], b_igate[l], b_fgate[l],
            g_mix_norm[l], g_mlstm_out[l], g_moba_q[l], g_moba_k[l], g_moba_out[l], g_cq[l], g_ckv[l],
            w_uq[l], w_ukv[l], g_mla_q[l], g_mla_k[l], g_mla_out[l], w_out[l], g_mlp_norm[l],
            w_up[l], w_down[l])
    return x
```

```python
from contextlib import ExitStack
import math

import numpy as np
import ml_dtypes

import concourse.bass as bass
import concourse.mybir as mybir
from concourse.bass_utils import run_bass_kernel_spmd

F32 = mybir.dt.float32
BF16 = mybir.dt.bfloat16
I32 = mybir.dt.int32
ALU = mybir.AluOpType
AF = mybir.ActivationFunctionType
AX = mybir.AxisListType

D_MODEL = 1024
SEQ = 8192
BATCH = 4
DEPTH = 2
D_FF = 4096
EPS = 1e-6
N_CORES = 8

DMA_RING = 6
ENGS = ("pe", "act", "dve", "pool", "sp")


class Prog:
    def __init__(self, nc, stack):
        self.nc = nc
        self.q = {e: [] for e in ENGS}
        self.sem = {e: stack.enter_context(nc.semaphore("s_" + e)) for e in ENGS}
        self.cnt = {e: 0 for e in ENGS}
        self.dsem = {e: [stack.enter_context(nc.semaphore("d_%s%d" % (e, i))) for i in range(DMA_RING)]
                     for e in ("sp", "pool", "act")}
        self.dn = {e: 0 for e in self.dsem}
        self.dlast = {e: [None] * DMA_RING for e in self.dsem}
        self.last_w = {}
        self.readers = {}
        self.known = {e: {} for e in ENGS}
        self.semobj = {}
        self.sb_off = 16512
        self.offs = {}
        self.last_off = 0
        self.n_t = 0

    def sb(self, shape, dtype, name=None, at=None):
        esz = {F32: 4, BF16: 2, I32: 4}[dtype]
        per = esz
        for s in shape[1:]:
            per *= s
        per = (per + 63) // 64 * 64
        self.n_t += 1
        off = self.sb_off if at is None else at
        t = self.nc.alloc_sbuf_tensor_at(name or ("t%d" % self.n_t), list(shape), dtype, offset=off)
        self.offs[t.name if hasattr(t, 'name') else id(t)] = off
        self.last_off = off
        if at is None:
            self.sb_off += per
            assert self.sb_off <= 229300, self.sb_off
        return t

    def _deps(self, eng, reads, writes):
        evs = []
        for t in reads:
            if t in self.last_w:
                evs.append(self.last_w[t])
        for t in writes:
            if t in self.last_w:
                evs.append(self.last_w[t])
            evs.extend(self.readers.get(t, ()))
        need = {}
        for (sk, v) in evs:
            if eng == "pe" and sk == "s_pe":
                continue
            if self.known[eng].get(sk, 0) >= v:
                continue
            if need.get(sk, 0) < v:
                need[sk] = v
        for sk, v in need.items():
            self.known[eng][sk] = v
        return list(need.items())

    def _record(self, ev, reads, writes):
        for t in writes:
            self.last_w[t] = ev
            self.readers[t] = []
        for t in reads:
            self.readers.setdefault(t, []).append(ev)

    def op(self, eng, fn, reads=(), writes=()):
        writes = list(writes) + [t for t in reads if isinstance(t, tuple) and t[0] == "ps" and t not in writes]
        waits = self._deps(eng, reads, writes)
        self.cnt[eng] += 1
        ev = ("s_" + eng, self.cnt[eng])
        self.semobj[ev[0]] = self.sem[eng]
        self.q[eng].append((waits, fn, self.sem[eng], 1))
        self._record(ev, reads, writes)

    def dma(self, queue, out, in_, reads=(), writes=(), **kw):
        n = self.dn[queue]
        slot = n % DMA_RING
        prev = self.dlast[queue][slot]
        waits = self._deps(queue, reads, writes)
        if prev is not None and self.known[queue].get(prev[0], 0) < prev[1]:
            waits = [w for w in waits if w[0] != prev[0]] + [prev]
            self.known[queue][prev[0]] = prev[1]
        sk = "d_%s%d" % (queue, slot)
        ev = (sk, 16 * (n // DMA_RING + 1))
        self.semobj[sk] = self.dsem[queue][slot]
        self.dlast[queue][slot] = ev
        self.dn[queue] += 1
        self.q[queue].append((waits, lambda e: e.dma_start(out=out, in_=in_, **kw), self.dsem[queue][slot], 16))
        self._record(ev, reads, writes)

    def finish(self):
        for queue in self.dsem:
            waits = []
            for ev in self.dlast[queue]:
                if ev is not None and self.known[queue].get(ev[0], 0) < ev[1]:
                    waits.append(ev)
            if waits:
                self.q[queue].append((waits, None, None, 0))

    def emit(self):
        nc = self.nc
        engobj = {"pe": nc.tensor, "act": nc.scalar, "dve": nc.vector, "pool": nc.gpsimd, "sp": nc.sync}

        def run(name, e):
            for waits, fn, sem, inc in self.q[name]:
                for sk, v in waits:
                    e.wait_ge(self.semobj[sk], v)
                if fn is not None:
                    fn(e).then_inc(sem, inc)

        with nc.Block() as block:
            @block.sync
            def _(eng):
                run("sp", eng)

            @block.tensor
            def _(eng):
                run("pe", eng)

            @block.scalar
            def _(eng):
                run("act", eng)

            @block.vector
            def _(eng):
                run("dve", eng)

            @block.gpsimd
            def _(eng):
                run("pool", eng)


def make_ident(p, ident_bf):
    onesf = p.sb([128, 128], F32)
    idf = p.sb([128, 128], F32)
    p.op("pool", lambda e: e.memset(onesf[:], 1.0), writes=["onesf"])
    p.op("pool", lambda e: e.affine_select(out=idf[:], in_=onesf[:], pattern=[[1, 128]],
                                           compare_op=ALU.is_equal, fill=0.0, base=0, channel_multiplier=-1),
         reads=["onesf"], writes=["idf"])
    p.op("pool", lambda e: e.tensor_copy(ident_bf[:], idf[:]), reads=["idf"], writes=["ident"])


CONV_ENGS = ("dve", "pool", "act")


def convert(p, i, out_ap, in_ap, reads, writes):
    eng = CONV_ENGS[i % 3]
    if eng == "act":
        p.op("act", lambda e: e.activation(out=out_ap, in_=in_ap, func=AF.Copy), reads=reads, writes=writes)
    else:
        p.op(eng, lambda e: e.tensor_copy(out_ap, in_ap), reads=reads, writes=writes)


def build_phase_c(ntok):
    nc = bass.Bass("TRN2", target_bir_lowering=False)
    x_in = nc.dram_tensor("x_in", [ntok, 1024], F32, kind="ExternalInput").ap()
    mixT = nc.dram_tensor("mixT", [1024, ntok], BF16, kind="ExternalInput").ap()
    w_out = nc.dram_tensor("w_out", [1024, 1024], F32, kind="ExternalInput").ap()
    w_up = nc.dram_tensor("w_up", [1024, 4096], F32, kind="ExternalInput").ap()
    w_down = nc.dram_tensor("w_down", [4096, 1024], F32, kind="ExternalInput").ap()
    g_mlp = nc.dram_tensor("g_mlp", [128, 8], F32, kind="ExternalInput").ap()
    x_out = nc.dram_tensor("x_out", [ntok, 1024], F32, kind="ExternalOutput").ap()
    x1buf = nc.dram_tensor("x1buf", [ntok, 1024], F32, kind="Internal").ap()
    with ExitStack() as stack:
        p = Prog(nc, stack)
        emit_phase_c(p, ntok, x_in, mixT, w_out, w_up, w_down, g_mlp, x_out, x1buf)
        p.finish()
        p.emit()
    return nc


def emit_phase_c(p, ntok, x_in, mixT, w_out, w_up, w_down, g_mlp, x_out, x1buf):
    nc = p.nc
    TT = 256
    ntile = ntok // TT
    wup = p.sb([128, 8, 4096], BF16)
    wdn = p.sb([128, 32, 1024], BF16)
    aT = p.sb([128, 32, TT], BF16)
    wo = p.sb([128, 8, 1024], BF16, at=p.last_off)
    xt = [p.sb([128, 2, 1024], F32) for _ in range(2)]
    h2T, mT = [], []
    for _ in range(2):
        h2T.append(p.sb([128, 8, TT], BF16))
        mT.append(p.sb([128, 8, TT], BF16, at=p.last_off))
    hb = p.sb([128, 2, 1024], BF16)
    junk = p.sb([128, 1024], BF16)
    rbuf = [p.sb([128, 512], BF16) for _ in range(2)]
    stg = [p.sb([128, 2048], F32) for _ in range(2)]
    g = p.sb([128, 8], F32)
    ssq = p.sb([128, 2], F32)
    lnv = p.sb([128, 2], F32)
    rstd = p.sb([128, 2], F32)
    ident = p.sb([128, 128], BF16)
    make_ident(p, ident)
    p.dma("sp", g[:], g_mlp, writes=["g"])

    ci = [0]

    def stage_convert(src_ap, dst_ap, wtok):
        i = ci[0]
        ci[0] += 1
        s = stg[i % 2]
        sv = s[:].rearrange("p (a b) -> p a b", a=src_ap.shape[1]) if len(src_ap.shape) == 3 else s[:]
        p.dma("sp", sv, src_ap, writes=[("stg", i % 2)])
        convert(p, i, dst_ap, sv, reads=[("stg", i % 2)], writes=[wtok])

    wo_v = w_out.rearrange("(c p) n -> p c n", p=128)
    for c in range(0, 8, 2):
        stage_convert(wo_v[:, c:c + 2, :], wo[:, c:c + 2, :], "wo")
    wu_v = w_up.rearrange("(c p) n -> p c n", p=128)
    for c in range(8):
        for hh in range(2):
            stage_convert(wu_v[:, c, hh * 2048:(hh + 1) * 2048], wup[:, c, hh * 2048:(hh + 1) * 2048], "wup")
    wd_v = w_down.rearrange("(c p) n -> p c n", p=128)
    for c in range(0, 32, 2):
        stage_convert(wd_v[:, c:c + 2, :], wdn[:, c:c + 2, :], "wdn")

    mixT_v = mixT.rearrange("(c p) t -> p c t", p=128)
    with ExitStack() as ps:
        py = [ps.enter_context(nc.psum_tensor("py%d" % i, [128, 512], F32)) for i in range(2)]
        ptr = [ps.enter_context(nc.psum_tensor("ptr%d" % i, [128, 1024], BF16)) for i in range(2)]
        pu = [ps.enter_context(nc.psum_tensor("pu%d" % i, [128, 2, TT], F32)) for i in range(2)]
        pcnt = {"y": 0, "tr": 0, "u": 0}

        for t in range(ntile):
            b = t % 2
            t0 = t * TT
            p.dma("sp", mT[b][:], mixT_v[:, :, t0:t0 + TT], writes=[("mT", b)])
            p.dma("sp", xt[b][:], x_in[t0:t0 + TT, :].rearrange("(s p) n -> p s n", p=128), writes=[("xt", b)])
            for s in range(2):
                for hf in range(2):
                    k = pcnt["y"] % 2
                    pcnt["y"] += 1

                    def mm(e, b=b, s=s, hf=hf, k=k):
                        for c in range(8):
                            r = e.matmul(py[k][:], lhsT=mT[b][:, c, s * 128:(s + 1) * 128],
                                         rhs=wo[:, c, hf * 512:(hf + 1) * 512], start=(c == 0), stop=(c == 7))
                        return r
                    p.op("pe", mm, reads=[("mT", b), "wo"], writes=[("py", k)])
                    p.op("dve", lambda e, b=b, s=s, hf=hf, k=k: e.tensor_tensor(
                        out=xt[b][:, s, hf * 512:(hf + 1) * 512], in0=py[k][:],
                        in1=xt[b][:, s, hf * 512:(hf + 1) * 512], op=ALU.add),
                        reads=[("py", k), ("xt", b)], writes=[("xt", b)])
            p.dma("pool", x1buf[t0:t0 + TT, :].rearrange("(s p) n -> p s n", p=128), xt[b][:],
                  reads=[("xt", b)], writes=["x1buf%d" % t])

        for t in range(ntile):
            b = t % 2
            t0 = t * TT
            p.dma("sp", xt[b][:], x1buf[t0:t0 + TT, :].rearrange("(s p) n -> p s n", p=128),
                  reads=["x1buf%d" % t], writes=[("xt", b)])
            for s in range(2):
                p.op("act", lambda e, b=b, s=s: e.activation(out=junk[:], in_=xt[b][:, s, :], func=AF.Square,
                                                             accum_out=ssq[:, s:s + 1]),
                     reads=[("xt", b)], writes=["junk", "ssq"])
            p.op("act", lambda e: e.activation(out=lnv[:], in_=ssq[:], func=AF.Ln, scale=1.0 / 1024, bias=EPS),
                 reads=["ssq"], writes=["lnv"])
            p.op("act", lambda e: e.activation(out=rstd[:], in_=lnv[:], func=AF.Exp, scale=-0.5),
                 reads=["lnv"], writes=["rstd"])
            for s in range(2):
                p.op("dve", lambda e, b=b, s=s: e.tensor_scalar(out=hb[:, s, :], in0=xt[b][:, s, :],
                                                                scalar1=rstd[:, s:s + 1], scalar2=None, op0=ALU.mult),
                     reads=[("xt", b), "rstd"], writes=["hb"])
            for c in range(8):
                k = pcnt["tr"] % 2
                pcnt["tr"] += 1

                def tr(e, c=c, k=k):
                    for s in range(2):
                        r = e.transpose(ptr[k][:, s * 128:(s + 1) * 128], hb[:, s, c * 128:(c + 1) * 128], ident[:])
                    return r
                p.op("pe", tr, reads=["hb", "ident"], writes=[("ptr", k)])
                if c % 2 == 0:
                    p.op("dve", lambda e, b=b, c=c, k=k: e.tensor_scalar(
                        out=h2T[b][:, c, :], in0=ptr[k][:, 0:TT], scalar1=g[:, c:c + 1], scalar2=None, op0=ALU.mult),
                        reads=[("ptr", k), "g"], writes=[("h2T", b)])
                else:
                    p.op("act", lambda e, b=b, c=c, k=k: e.activation(
                        out=h2T[b][:, c, :], in_=ptr[k][:, 0:TT], func=AF.Copy, scale=g[:, c:c + 1]),
                        reads=[("ptr", k), "g"], writes=[("h2T", b)])
            for f2 in range(16):
                k = pcnt["u"] % 2
                pcnt["u"] += 1

                def up(e, b=b, f2=f2, k=k):
                    for j in range(2):
                        f = f2 * 2 + j
                        for c in range(8):
                            r = e.matmul(pu[k][:, j, :], lhsT=wup[:, c, f * 128:(f + 1) * 128], rhs=h2T[b][:, c, :],
                                         start=(c == 0), stop=(c == 7))
                    return r
                p.op("pe", up, reads=[("h2T", b), "wup"], writes=[("pu", k)])
                p.op("act", lambda e, k=k: e.activation(out=rbuf[k][:], in_=pu[k][:].rearrange("p a b -> p (a b)"),
                                                        func=AF.Relu),
                     reads=[("pu", k)], writes=[("rbuf", k)])
                p.op("pool", lambda e, f2=f2, k=k: e.tensor_tensor(
                    out=aT[:, f2 * 2:f2 * 2 + 2, :], in0=rbuf[k][:].rearrange("p (a b) -> p a b", a=2),
                    in1=rbuf[k][:].rearrange("p (a b) -> p a b", a=2), op=ALU.mult),
                    reads=[("rbuf", k)], writes=["aT"])
            for s in range(2):
                for hf in range(2):
                    k = pcnt["y"] % 2
                    pcnt["y"] += 1

                    def dn(e, s=s, hf=hf, k=k):
                        for f in range(32):
                            r = e.matmul(py[k][:], lhsT=aT[:, f, s * 128:(s + 1) * 128],
                                         rhs=wdn[:, f, hf * 512:(hf + 1) * 512], start=(f == 0), stop=(f == 31))
                        return r
                    p.op("pe", dn, reads=["aT", "wdn"], writes=[("py", k)])
                    p.op("dve", lambda e, b=b, s=s, hf=hf, k=k: e.tensor_tensor(
                        out=xt[b][:, s, hf * 512:(hf + 1) * 512], in0=py[k][:],
                        in1=xt[b][:, s, hf * 512:(hf + 1) * 512], op=ALU.add),
                        reads=[("py", k), ("xt", b)], writes=[("xt", b)])
            p.dma("pool", x_out[t0:t0 + TT, :].rearrange("(s p) n -> p s n", p=128), xt[b][:],
                  reads=[("xt", b)], writes=["x_out"])


def op_tt(p, eng, out, in0, in1, op, r, w):
    p.op(eng, lambda e: e.tensor_tensor(out=out, in0=in0, in1=in1, op=op), reads=r, writes=w)


def op_ts(p, eng, out, in0, s1, op0, r, w, s2=None, op1=None):
    if op1 is None:
        p.op(eng, lambda e: e.tensor_scalar(out=out, in0=in0, scalar1=s1, scalar2=None, op0=op0), reads=r, writes=w)
    else:
        p.op(eng, lambda e: e.tensor_scalar(out=out, in0=in0, scalar1=s1, scalar2=s2, op0=op0, op1=op1),
             reads=r, writes=w)


def op_stt(p, out, in0, scalar, in1, op0, op1, r, w):
    p.op("dve", lambda e: e.scalar_tensor_tensor(out=out, in0=in0, scalar=scalar, in1=in1, op0=op0, op1=op1),
         reads=r, writes=w)


def op_act(p, out, in_, func, r, w, scale=1.0, bias=None, accum=None):
    kw = {}
    if bias is not None:
        kw["bias"] = bias
    if accum is not None:
        kw["accum_out"] = accum
    p.op("act", lambda e: e.activation(out=out, in_=in_, func=func, scale=scale, **kw), reads=r, writes=w)


def op_copy(p, eng, out, in_, r, w):
    if eng == "act":
        p.op("act", lambda e: e.activation(out=out, in_=in_, func=AF.Copy), reads=r, writes=w)
    else:
        p.op(eng, lambda e: e.tensor_copy(out, in_), reads=r, writes=w)


def barrier(p):
    evs = [("s_" + e, p.cnt[e]) for e in ENGS if p.cnt[e] > 0]
    for q in p.dsem:
        for ev in p.dlast[q]:
            if ev is not None:
                evs.append(ev)
    for e in ENGS:
        need = {}
        for sk, v in evs:
            if p.known[e].get(sk, 0) < v and need.get(sk, 0) < v:
                need[sk] = v
        for sk, v in need.items():
            p.known[e][sk] = v
        if need:
            p.q[e].append((list(need.items()), None, None, 0))


def rms_groups(p, tag, X, G, ngrp, D, sqj, ss, lnv, rs):
    op_tt(p, "pool", sqj, X, X, ALU.mult, [tag], [tag + "sq"])
    p.op("dve", lambda e: e.tensor_reduce(out=ss, in_=sqj, axis=AX.X, op=ALU.add), reads=[tag + "sq"], writes=[tag + "ss"])
    op_act(p, lnv, ss, AF.Ln, [tag + "ss"], [tag + "ln"], scale=1.0 / D, bias=EPS)
    op_act(p, rs, lnv, AF.Exp, [tag + "ln"], [tag + "rs"], scale=-0.5)
    op_tt(p, "pool", X, X, G, ALU.mult, [tag, "gains"], [tag])
    op_tt(p, "dve", X, X, rs.unsqueeze(2).to_broadcast([128, ngrp, D]), ALU.mult, [tag, tag + "rs"], [tag])


def rope_apply(p, tag, Y, OUT, a, r, cos, sin, shp, tmp):
    x1 = Y[:, :, :, a:a + r]
    x2 = Y[:, :, :, a + r:a + 2 * r]
    t1, t2, t3, t4 = [tmp[:, i].rearrange("p (a b r) -> p a b r", a=shp[1], b=shp[2]) for i in range(4)]
    op_tt(p, "dve", t1, x1, cos, ALU.mult, [tag, "rope"], [tag + "t1"])
    op_tt(p, "pool", t2, x2, sin, ALU.mult, [tag, "rope"], [tag + "t2"])
    op_tt(p, "dve", t3, x2, cos, ALU.mult, [tag, "rope"], [tag + "t3"])
    op_tt(p, "pool", t4, x1, sin, ALU.mult, [tag, "rope"], [tag + "t4"])
    op_tt(p, "dve", OUT[:, :, :, a:a + r], t1, t2, ALU.subtract, [tag + "t1", tag + "t2"], [tag + "o"])
    op_tt(p, "pool", OUT[:, :, :, a + r:a + 2 * r], t3, t4, ALU.add, [tag + "t3", tag + "t4"], [tag + "o"])


NFM = 900
NTM = 928
SC_MLA = 96 ** -0.5
SC_MOBA = 64 ** -0.5
TWO_PI = 2.0 * math.pi
CW1 = 6.28125
CW2 = TWO_PI - CW1


def b_dram(nc, S):
    d = {}

    def inp(name, shape, dt=F32):
        d[name] = nc.dram_tensor(name, shape, dt, kind="ExternalInput").ap()

    def scr(name, shape, dt=BF16):
        d[name] = nc.dram_tensor(name, shape, dt, kind="Internal").ap()
    inp("x", [S, 1024])
    inp("pos", [128, S // 128], I32)
    inp("g_mix", [128, 8])
    inp("w_fm", [1024, NFM])
    inp("w_tm", [1024, NTM])
    inp("conv_w", [128, 2, 4])
    inp("conv_b", [128, 2])
    inp("gate_b", [2, 2])
    inp("g_mout", [128, 2])
    inp("g_moba_qk", [128, 256])
    inp("g_moba_out", [64, 2])
    inp("g_cq", [128, 3])
    inp("g_ckv", [128, 2])
    inp("w_uq", [384, 192])
    inp("w_ukv", [256, 256])
    inp("g_mla_q", [128, 96])
    inp("g_mla_k", [128, 96])
    inp("g_mla_out", [64, 2])
    inp("invf", [128, 24])
    d["mixT"] = nc.dram_tensor("mixT", [512, S], BF16, kind="ExternalOutput").ap()
    scr("mqT", [128, S]); scr("mkT", [128, S]); scr("mv", [S // 512, 128, 4 * 256]); scr("mo", [S // 512, 128, 4 * 256])
    scr("gi", [2, S], F32); scr("gf", [2, S], F32)
    scr("aqT", [128, S]); scr("akT", [128, S]); scr("av", [S // 512, 128, 4 * 132])
    scr("lqT", [2, 96, S]); scr("lkT", [2, 96, S]); scr("lv", [S // 512, 128, 4 * 132])
    scr("kmT", [128, S // 256], F32)
    return d


def build_phase_b(S, parts=("b1", "mla", "moba", "mlstm")):
    nc = bass.Bass("TRN2", target_bir_lowering=False)
    d = b_dram(nc, S)
    with ExitStack() as stack:
        p = Prog(nc, stack)
        ps = [stack.enter_context(nc.psum_tensor("ps%d" % i, [128, 512], F32)) for i in range(8)]
        emit_phase_b(p, S, d, ps, parts)
        p.finish()
        p.emit()
    return nc


def emit_phase_b(p, S, d, ps, parts):
    base = p.sb_off
    ident = p.sb([128, 128], BF16)
    make_ident(p, ident)
    base2 = p.sb_off
    if "b1" in parts:
        emit_b1(p, S, d, ps, ident)
        barrier(p)
    p.sb_off = base2
    if "mla" in parts:
        emit_attn(p, S, d, ps, "mla")
        barrier(p)
    p.sb_off = base2
    if "moba" in parts:
        emit_attn(p, S, d, ps, "moba")
        barrier(p)
    p.sb_off = base2
    if "mlstm" in parts:
        emit_mlstm(p, S, d, ps, ident)


def emit_rope_tables(p, S, d, cs_p, cs_d):
    NT = S // 128
    posi = p.sb([128, NT], I32)
    posf = p.sb([128, NT], F32)
    invf = p.sb([128, 24], F32)
    p.dma("sp", posi[:], d["pos"], writes=["posi"])
    p.dma("sp", invf[:], d["invf"], writes=["invf"])
    op_copy(p, "dve", posf[:], posi[:], ["posi"], ["posf"])
    for (cs, f0, R) in ((cs_p, 0, 8), (cs_d, 8, 16)):
        ang = p.sb([128, NT, R], F32)
        y = p.sb([128, NT, R], F32)
        kf = p.sb([128, NT, R], F32)
        ki = p.sb([128, NT, R], I32)
        c1 = p.sb([128, NT, R], F32)
        tg = "rt%d" % R
        op_tt(p, "dve", ang[:], posf[:].unsqueeze(2).to_broadcast([128, NT, R]),
              invf[:, f0:f0 + R].unsqueeze(1).to_broadcast([128, NT, R]), ALU.mult, ["posf", "invf"], [tg + "ang"])
        for which, phi in ((0, math.pi / 2), (1, 0.0)):
            op_ts(p, "dve", y[:], ang[:], phi, ALU.add, [tg + "ang"], [tg + "y"])
            op_ts(p, "dve", kf[:], y[:], 1.0 / TWO_PI, ALU.mult, [tg + "y"], [tg + "kf"], s2=0.5, op1=ALU.add)
            op_copy(p, "dve", ki[:], kf[:], [tg + "kf"], [tg + "ki"])
            op_copy(p, "dve", kf[:], ki[:], [tg + "ki"], [tg + "kf"])
            op_stt(p, y[:], kf[:], -CW1, y[:], ALU.mult, ALU.add, [tg + "kf", tg + "y"], [tg + "y"])
            op_stt(p, y[:], kf[:], -CW2, y[:], ALU.mult, ALU.add, [tg + "kf", tg + "y"], [tg + "y"])
            op_ts(p, "dve", c1[:], y[:], math.pi, ALU.is_gt, [tg + "y"], [tg + "c1"])
            op_stt(p, y[:], c1[:], -TWO_PI, y[:], ALU.mult, ALU.add, [tg + "c1", tg + "y"], [tg + "y"])
            op_ts(p, "dve", c1[:], y[:], -math.pi, ALU.is_lt, [tg + "y"], [tg + "c1"])
            op_stt(p, y[:], c1[:], TWO_PI, y[:], ALU.mult, ALU.add, [tg + "c1", tg + "y"], [tg + "y"])
            op_ts(p, "dve", y[:], y[:], math.pi, ALU.min, [tg + "y"], [tg + "y"], s2=-math.pi, op1=ALU.max)
            op_act(p, cs[:, :, which, :], y[:], AF.Sin, [tg + "y"], ["rope"])


def emit_b1(p, S, d, ps, ident):
    import os
    STOP = int(os.environ.get('B1_STOP', '99'))
    nc = p.nc
    NTL = S // 512
    psb = [t[:].bitcast(BF16) for t in ps]
    cs_p = p.sb([128, S // 128, 2, 8], F32)
    cs_d = p.sb([128, S // 128, 2, 16], F32)
    mark = p.sb_off
    emit_rope_tables(p, S, d, cs_p, cs_d)
    barrier(p)
    if STOP < 1:
        return
    p.sb_off = mark
    wfm = p.sb([128, 8, NFM], BF16)
    wtm = p.sb([128, 8, NTM], BF16)
    wuq = p.sb([128, 3, 192], BF16)
    wukv = p.sb([128, 2, 256], BF16)
    gmix = p.sb([128, 8], F32)
    cw = p.sb([128, 2, 4], F32)
    cb = p.sb([128, 2], F32)
    gb = p.sb([2, 2], F32)
    gqk = p.sb([128, 256], F32)
    gcq = p.sb([128, 3], F32)
    gckv = p.sb([128, 2], F32)
    gmq = p.sb([128, 96], F32)
    gmk = p.sb([128, 96], F32)
    ones_bf = p.sb([128, 1], BF16)
    for t, nm in ((gmix, "g_mix"), (cw, "conv_w"), (cb, "conv_b"), (gb, "gate_b"), (gqk, "g_moba_qk"),
                  (gcq, "g_cq"), (gckv, "g_ckv"), (gmq, "g_mla_q"), (gmk, "g_mla_k")):
        p.dma("sp", t[:], d[nm], writes=["gains"])
    p.op("pool", lambda e: e.memset(ones_bf[:], 1.0), writes=["ones"])
    stg = [p.sb([128, 2048], F32) for _ in range(2)]
    ci = [0]

    def stage(src, dst, n, wtok, scale=None):
        i = ci[0]
        ci[0] += 1
        sv = stg[i % 2][:, 0:n]
        p.dma("sp", sv, src, writes=[("stg", i % 2)])
        if scale is None:
            convert(p, i, dst, sv, [("stg", i % 2)], [wtok])
        else:
            op_ts(p, "dve", dst, sv, scale, ALU.mult, [("stg", i % 2), "gains"], [wtok])
    wfm_v = d["w_fm"].rearrange("(c p) n -> p c n", p=128)
    wtm_v = d["w_tm"].rearrange("(c p) n -> p c n", p=128)
    for c in range(8):
        stage(wfm_v[:, c, :], wfm[:, c, :], NFM, "wfm")
        stage(wtm_v[:, c, :], wtm[:, c, :], NTM, "wtm")
    wgate = p.sb([128, 8, 2, 16], BF16)
    op_copy(p, "dve", wgate[:, :, :, 0:2], wfm[:, :, 896:900].rearrange("p c (g t) -> p c g t", g=2), ["wfm"], ["wgate"])
    wuq_v = d["w_uq"].rearrange("(c p) n -> p c n", p=128)
    for c in range(3):
        stage(wuq_v[:, c, :], wuq[:, c, :], 192, "wuq", scale=gcq[:, c:c + 1])
    wukv_v = d["w_ukv"].rearrange("(c p) n -> p c n", p=128)
    for c in range(2):
        stage(wukv_v[:, c, :], wukv[:, c, :], 256, "wukv", scale=gckv[:, c:c + 1])

    if STOP < 2:
        return
    xt = [p.sb([128, 4, 1024], F32) for _ in range(2)]
    hb = p.sb([128, 4, 1024], BF16)
    junk = p.sb([128, 1024], BF16)
    hT = p.sb([128, 8, 512], BF16)
    ssq = p.sb([128, 4], F32)
    lnx = p.sb([128, 4], F32)
    rstd = p.sb([128, 4], F32)
    qkpre = [p.sb([128, 515], F32) for _ in range(2)]
    cacc = [p.sb([128, 512], F32) for _ in range(2)]
    qk_o = [p.sb([128, 512], BF16) for _ in range(2)]
    cqT = p.sb([128, 3, 512], BF16)
    cqsq = p.sb([128, 3, 512], BF16)
    ckvT = p.sb([128, 2, 512], BF16)
    ckvsq = p.sb([128, 2, 512], BF16)
    gst = p.sb([2, 2, 512], F32)
    mo_t = p.sb([128, 4, 256], BF16)
    mv_t = p.sb([128, 4, 256], BF16)
    av_t = p.sb([128, 4, 2, 66], BF16)
    lv_t = p.sb([128, 4, 2, 66], BF16)
    tmst = p.sb([128, 4, 256], F32)
    tms2 = p.sb([128, 4, 160], F32)
    zst = p.sb([128, 4, 450], F32)
    sqj = p.sb([128, 4 * 256], F32)
    ss16 = p.sb([128, 16], F32)
    ln16 = p.sb([128, 16], F32)
    rs16 = p.sb([128, 16], F32)
    QK = p.sb([128, 4, 4, 64], BF16)
    ropet = p.sb([128, 4, 4 * 4 * 16], F32)
    rc = p.sb([128, 4, 2], F32)
    lnc = p.sb([128, 4, 2], F32)
    ZQ = p.sb([128, 4, 2, 96], F32)
    KL = p.sb([128, 4, 2, 96], F32)
    LQ = p.sb([128, 4, 2, 96], BF16)
    LK = p.sb([128, 4, 2, 96], BF16)
    ss8 = p.sb([128, 8], F32)
    ln8 = p.sb([128, 8], F32)
    rs8 = p.sb([128, 8], F32)
    aq_o = p.sb([128, 512], BF16)
    ak_o = p.sb([128, 512], BF16)
    lq_o = p.sb([96, 2, 512], BF16)
    lk_o = p.sb([96, 2, 512], BF16)
    kmT = p.sb([128, max(S // 256, 1)], F32)
    for q_ in qkpre:
        p.op("pool", lambda e, q_=q_: e.memset(q_[:, 0:3], 0.0), writes=["qkpre"])
    p.op("pool", lambda e: e.memset(av_t[:], 1.0), writes=["av_t"])
    p.op("pool", lambda e: e.memset(lv_t[:], 1.0), writes=["lv_t"])

    pc = {"tr": 0, "fm": 0, "tm": 0}

    def bank(kind):
        lo = {"tr": 0, "fm": 2, "tm": 4}[kind]
        k = lo + pc[kind] % 2
        pc[kind] += 1
        return k

    x_v = d["x"]
    for T in range(NTL):
        b = T % 2
        t0 = T * 512
        if T == 0:
            p.dma("sp", xt[0][:], x_v[0:512, :].rearrange("(s p) n -> p s n", p=128), writes=[("xt", 0)])
        if T + 1 < NTL:
            p.dma("sp", xt[1 - b][:], x_v[t0 + 512:t0 + 1024, :].rearrange("(s p) n -> p s n", p=128),
                  writes=[("xt", 1 - b)])
        for s in range(4):
            op_act(p, junk[:], xt[b][:, s, :], AF.Square, [("xt", b)], ["junk", "ssq"], accum=ssq[:, s:s + 1])
        op_act(p, lnx[:], ssq[:], AF.Ln, ["ssq"], ["lnx"], scale=1.0 / 1024, bias=EPS)
        op_act(p, rstd[:], lnx[:], AF.Exp, ["lnx"], ["rstd"], scale=-0.5)
        for s in range(4):
            op_ts(p, "dve" if s % 2 == 0 else "pool", hb[:, s, :], xt[b][:, s, :], rstd[:, s:s + 1], ALU.mult,
                  [("xt", b), "rstd"], ["hb%d" % s])
        for c in range(8):
            k = bank("tr")

            def tr(e, c=c, k=k):
                for s in range(4):
                    r = e.transpose(psb[k][:, s * 128:(s + 1) * 128], hb[:, s, c * 128:(c + 1) * 128], ident[:])
                return r
            p.op("pe", tr, reads=["hb0", "hb1", "hb2", "hb3", "ident"], writes=[("ps", k)])
            if c % 2 == 0:
                op_ts(p, "dve", hT[:, c, :], psb[k][:, 0:512], gmix[:, c:c + 1], ALU.mult, [("ps", k), "gains"], ["hT"])
            else:
                op_act(p, hT[:, c, :], psb[k][:, 0:512], AF.Copy, [("ps", k), "gains"], ["hT"], scale=gmix[:, c:c + 1])

        if STOP < 3:
            continue
        for m in range(7):
            k = bank("fm")

            def fm(e, m=m, k=k):
                for c in range(8):
                    r = e.matmul(ps[k][:], lhsT=wfm[:, c, m * 128:(m + 1) * 128], rhs=hT[:, c, :],
                                 start=(c == 0), stop=(c == 7))
                return r
            p.op("pe", fm, reads=["hT", "wfm"], writes=[("ps", k)])
            if m < 2:
                qp = qkpre[m]
                tg = "qkpre%d" % m
                op_copy(p, "act", qp[:, 3:515], ps[k][:], [("ps", k)], [tg])
                op_ts(p, "dve", cacc[m][:], qp[:, 0:512], cw[:, m, 0:1], ALU.mult, [tg, "gains"], ["cacc%d" % m])
                for j in range(1, 4):
                    op_stt(p, cacc[m][:], qp[:, j:j + 512], cw[:, m, j:j + 1], cacc[m][:], ALU.mult, ALU.add,
                           [tg, "gains", "cacc%d" % m], ["cacc%d" % m])
                op_copy(p, "pool", qp[:, 0:3], qp[:, 512:515], [tg], [tg])
                if m == 0:
                    op_act(p, cacc[m][:], cacc[m][:], AF.Silu, ["cacc0", "gains"], ["cacc0"], bias=cb[:, 0:1])
                    op_ts(p, "dve", qk_o[0][:], cacc[0][:], 0.125, ALU.mult, ["cacc0"], ["qk_o0"])
                    p.dma("sp", d["mqT"][:, t0:t0 + 512], qk_o[0][:], reads=["qk_o0"], writes=["mqT"])
                else:
                    op_act(p, qk_o[1][:], cacc[m][:], AF.Silu, ["cacc1", "gains"], ["qk_o1"], bias=cb[:, 1:2])
                    p.dma("sp", d["mkT"][:, t0:t0 + 512], qk_o[1][:], reads=["qk_o1"], writes=["mkT"])
            elif m < 5:
                op_copy(p, "act", cqT[:, m - 2, :], ps[k][:], [("ps", k)], ["cqT"])
                op_tt(p, "pool", cqsq[:, m - 2, :], cqT[:, m - 2, :], cqT[:, m - 2, :], ALU.mult, ["cqT"], ["cqsq"])
            else:
                op_copy(p, "act", ckvT[:, m - 5, :], ps[k][:], [("ps", k)], ["ckvT"])
                op_tt(p, "pool", ckvsq[:, m - 5, :], ckvT[:, m - 5, :], ckvT[:, m - 5, :], ALU.mult, ["ckvT"], ["ckvsq"])
        if STOP < 4:
            continue
        for gi_ in range(2):
            k = bank("tm")

            def gm(e, gi_=gi_, k=k):
                for c in range(8):
                    r = e.matmul(ps[k][0:2, 0:512], lhsT=wgate[:, c, gi_, 0:2], rhs=hT[:, c, :],
                                 start=(c == 0), stop=(c == 7))
                return r
            p.op("pe", gm, reads=["hT", "wgate"], writes=[("ps", k)])
            GD = int(os.environ.get("GATE_DBG", "0"))
            if GD == 2:
                continue
            op_act(p, gst[:, gi_, :], ps[k][0:2, 0:512], AF.Identity, [("ps", k), "gains"], ["gst%d" % gi_],
                   bias=gb[:, gi_:gi_ + 1])
            if GD == 1:
                continue
            p.dma("sp", d["gi" if gi_ == 0 else "gf"][:, t0:t0 + 512], gst[:, gi_, :], reads=["gst%d" % gi_],
                  writes=["gates"])

        if STOP < 5:
            continue
        for s in range(4):
            k = bank("tm")

            def tm1(e, s=s, k=k):
                for c in range(8):
                    r = e.matmul(ps[k][:], lhsT=hT[:, c, s * 128:(s + 1) * 128], rhs=wtm[:, c, 0:512],
                                 start=(c == 0), stop=(c == 7))
                return r
            p.op("pe", tm1, reads=["hT", "wtm"], writes=[("ps", k)])
            XD = int(os.environ.get("X_DBG", "0"))
            op_act(p, mo_t[:, s, :], ps[k][:, 256:512], AF.Identity if XD == 1 else AF.Sigmoid, [("ps", k)], ["mo_t"])
            if XD != 2:
                op_copy(p, "dve", mv_t[:, s, :], ps[k][:, 0:256], [("ps", k)], ["mv_t"])
            TMD = int(os.environ.get("TM_DBG", "9"))
            if TMD < 2:
                continue
            k = bank("tm")

            def tm2(e, s=s, k=k):
                for c in range(8):
                    r = e.matmul(ps[k][:, 0:416], lhsT=hT[:, c, s * 128:(s + 1) * 128], rhs=wtm[:, c, 512:928],
                                 start=(c == 0), stop=(c == 7))
                return r
            p.op("pe", tm2, reads=["hT", "wtm"], writes=[("ps", k)])
            op_copy(p, "dve", tmst[:, s, :], ps[k][:, 0:256], [("ps", k)], ["tmst"])
            op_copy(p, "act", tms2[:, s, :], ps[k][:, 256:416], [("ps", k)], ["tms2"])

            if TMD < 3:
                continue

            def zz(e, s=s):
                sl = slice(s * 128, (s + 1) * 128)
                for c in range(3):
                    e.matmul(ps[6][:, 0:192], lhsT=cqT[:, c, sl], rhs=wuq[:, c, :], start=(c == 0), stop=(c == 2))
                for c in range(2):
                    e.matmul(ps[6][:, 192:448], lhsT=ckvT[:, c, sl], rhs=wukv[:, c, :], start=(c == 0), stop=(c == 1))
                for c in range(3):
                    e.matmul(ps[6][:, 448:449], lhsT=cqsq[:, c, sl], rhs=ones_bf[:], start=(c == 0), stop=(c == 2))
                for c in range(2):
                    r = e.matmul(ps[6][:, 449:450], lhsT=ckvsq[:, c, sl], rhs=ones_bf[:], start=(c == 0), stop=(c == 1))
                return r
            p.op("pe", zz, reads=["cqT", "ckvT", "cqsq", "ckvsq", "wuq", "wukv", "ones"], writes=[("ps", 6)])
            op_copy(p, "act", zst[:, s, :], ps[6][:, 0:450], [("ps", 6)], ["zst"])
        p.dma("sp", d["mo"][T], mo_t[:].rearrange("p s n -> p (s n)"), reads=["mo_t"], writes=["mo"])
        if int(os.environ.get("MV_DBG", "1")):
            p.dma("sp", d["mv"][T], mv_t[:].rearrange("p s n -> p (s n)"), reads=["mv_t"], writes=["mv"])

        if STOP < 6:
            continue
        X = tmst[:]
        Xg = X.rearrange("p s (v d) -> p (s v) d", v=4)
        op_tt(p, "pool", sqj[:].rearrange("p (s n) -> p s n", s=4), X, X, ALU.mult, ["tmst"], ["sqj"])
        p.op("dve", lambda e: e.tensor_reduce(out=ss16[:], in_=sqj[:].rearrange("p (g d) -> p g d", d=64),
                                              axis=AX.X, op=ALU.add), reads=["sqj"], writes=["ss16"])
        op_act(p, ln16[:], ss16[:], AF.Ln, ["ss16"], ["ln16"], scale=1.0 / 64, bias=EPS)
        op_act(p, rs16[:], ln16[:], AF.Exp, ["ln16"], ["rs16"], scale=-0.5)
        op_tt(p, "pool", X, X, gqk[:].unsqueeze(1).to_broadcast([128, 4, 256]), ALU.mult, ["tmst", "gains"], ["tmst"])
        op_tt(p, "dve", Xg, Xg, rs16[:].unsqueeze(2).to_broadcast([128, 16, 64]), ALU.mult, ["tmst", "rs16"], ["tmst"])
        X4 = X.rearrange("p s (v d) -> p s v d", v=4)
        cosb = cs_p[:, T * 4:T * 4 + 4, 0, :].unsqueeze(2).to_broadcast([128, 4, 4, 8])
        sinb = cs_p[:, T * 4:T * 4 + 4, 1, :].unsqueeze(2).to_broadcast([128, 4, 4, 8])
        rope_apply(p, "tmst", X4, QK[:], 0, 8, cosb, sinb, [128, 4, 4, 8], ropet[:, :, 0:128])
        op_copy(p, "act", QK[:, :, :, 16:64], X4[:, :, :, 16:64], ["tmst"], ["tmsto"])
        op_copy(p, "dve", av_t[:, :, :, 0:64], tms2[:, :, 0:128].rearrange("p s (h d) -> p s h d", h=2),
                ["tms2"], ["av_t"])
        p.dma("sp", d["av"][T], av_t[:].rearrange("p s h d -> p (s h d)"), reads=["av_t"], writes=["av"])
        if STOP < 7:
            continue
        for blk in range(2):
            n = T * 2 + blk

            def km(e, blk=blk, n=n):
                for s2 in range(2):
                    r = e.matmul(ps[7][:, n % 512:n % 512 + 1],
                                 lhsT=QK[:, blk * 2 + s2, 2:4, :].rearrange("p v d -> p (v d)"), rhs=ones_bf[:],
                                 start=(s2 == 0), stop=(s2 == 1))
                return r
            p.op("pe", km, reads=["tmsto", "ones"], writes=[("ps", 7)])
            op_ts(p, "dve", kmT[:, n:n + 1], ps[7][:, n % 512:n % 512 + 1], 1.0 / 256, ALU.mult, [("ps", 7)], ["kmT"])
        for which, o_t, dn in ((0, aq_o, "aqT"), (1, ak_o, "akT")):
            k = bank("tr")

            def trq(e, which=which, k=k):
                for s in range(4):
                    r = e.transpose(psb[k][:, s * 128:(s + 1) * 128],
                                    QK[:, s, 2 * which:2 * which + 2, :].rearrange("p v d -> p (v d)"), ident[:])
                return r
            p.op("pe", trq, reads=["tmsto", "ident"], writes=[("ps", k)])
            op_copy(p, "act" if which == 0 else "dve", o_t[:], psb[k][:, 0:512], [("ps", k)], [dn + "_o"])
            p.dma("sp", d[dn][:, t0:t0 + 512], o_t[:], reads=[dn + "_o"], writes=[dn])

        if STOP < 8:
            continue
        for j, dd in ((0, 384), (1, 256)):
            op_act(p, lnc[:, :, j:j + 1], zst[:, :, 448 + j:449 + j], AF.Ln, ["zst"], ["lnc"], scale=1.0 / dd, bias=EPS)
        op_act(p, rc[:], lnc[:], AF.Exp, ["lnc"], ["rc"], scale=-0.5)
        zv = zst[:, :, 0:192].rearrange("p s (h d) -> p s h d", h=2)
        kvv = zst[:, :, 192:448].rearrange("p s (h d) -> p s h d", h=2)
        op_tt(p, "dve", ZQ[:].rearrange("p s h d -> p s (h d)"), zst[:, :, 0:192],
              rc[:, :, 0:1].to_broadcast([128, 4, 192]), ALU.mult, ["zst", "rc"], ["ZQ"])
        op_tt(p, "dve", KL[:, :, :, 0:64], kvv[:, :, :, 0:64],
              rc[:, :, 1:2].unsqueeze(3).to_broadcast([128, 4, 2, 64]), ALU.mult, ["zst", "rc"], ["KL"])
        op_copy(p, "pool", KL[:, :, :, 64:96], tms2[:, :, 128:160].unsqueeze(2).to_broadcast([128, 4, 2, 32]),
                ["tms2"], ["KL"])
        op_tt(p, "dve", lv_t[:, :, :, 0:64], kvv[:, :, :, 64:128],
              rc[:, :, 1:2].unsqueeze(3).to_broadcast([128, 4, 2, 64]), ALU.mult, ["zst", "rc"], ["lv_t"])
        p.dma("sp", d["lv"][T], lv_t[:].rearrange("p s h d -> p (s h d)"), reads=["lv_t"], writes=["lv"])
        cosd = cs_d[:, T * 4:T * 4 + 4, 0, :].unsqueeze(2).to_broadcast([128, 4, 2, 16])
        sind = cs_d[:, T * 4:T * 4 + 4, 1, :].unsqueeze(2).to_broadcast([128, 4, 2, 16])
        for (Xt, Ot, gt, tg, o_t, dn) in ((ZQ, LQ, gmq, "ZQ", lq_o, "lqT"), (KL, LK, gmk, "KL", lk_o, "lkT")):
            rms_groups(p, tg, Xt[:].rearrange("p s h d -> p (s h) d"),
                       gt[:].unsqueeze(1).to_broadcast([128, 8, 96]), 8, 96,
                       sqj[:, 0:768].rearrange("p (g d) -> p g d", d=96), ss8[:], ln8[:], rs8[:])
            rope_apply(p, tg, Xt[:], Ot[:], 64, 16, cosd, sind, [128, 4, 2, 16], ropet[:, :, 0:128])
            op_copy(p, "act", Ot[:, :, :, 0:64], Xt[:, :, :, 0:64], [tg], [tg + "o"])
            for h in range(2):
                k = bank("tr")

                def trl(e, h=h, k=k, Ot=Ot):
                    for s in range(4):
                        r = e.transpose(psb[k][0:96, s * 128:(s + 1) * 128], Ot[:, s, h, :], ident[:])
                    return r
                p.op("pe", trl, reads=[tg + "o", "ident"], writes=[("ps", k)])
                op_copy(p, "act" if h == 0 else "dve", o_t[:, h, :], psb[k][0:96, 0:512], [("ps", k)], [dn + "_o"])
            p.dma("sp", d[dn][:, :, t0:t0 + 512].rearrange("h r t -> r h t"), o_t[:], reads=[dn + "_o"], writes=[dn])
    p.dma("sp", d["kmT"], kmT[:], reads=["kmT"], writes=["kmTd"])


def rope_inv_freq():
    def f(dim, theta):
        return (np.float32(theta) ** (-np.arange(0, dim, 2, dtype=np.float32) / np.float32(dim))).astype(np.float32)
    return np.concatenate([f(16, 500000.0), f(32, 10000.0)]).astype(np.float32)


def prep_b_weights(W, l, j):
    w_in = W["w_in"][l]
    cols_fm = np.concatenate([np.arange(128) + 128 * j, 256 + np.arange(128) + 128 * j, 2312 + np.arange(384),
                              2696 + np.arange(256), 1536 + 2 * j + np.arange(2), 1540 + 2 * j + np.arange(2)])
    cols_tm = np.concatenate([512 + 256 * j + np.arange(256), 1024 + 256 * j + np.arange(256),
                              1544 + 128 * j + np.arange(128), 1544 + 256 + 128 * j + np.arange(128),
                              1544 + 512 + 128 * j + np.arange(128), 2952 + np.arange(32)])
    cw = W["conv_w"][l]
    ch = np.stack([np.arange(128) + 128 * j, 256 + np.arange(128) + 128 * j], 0)
    rep = lambda v: np.ascontiguousarray(np.broadcast_to(v[None, :], (128, v.shape[0])))
    gq, gk = W["g_moba_q"][l], W["g_moba_k"][l]
    return {
        "g_mix": np.ascontiguousarray(W["g_mix_norm"][l].reshape(8, 128).T),
        "w_fm": np.ascontiguousarray(w_in[:, cols_fm]),
        "w_tm": np.ascontiguousarray(w_in[:, cols_tm]),
        "conv_w": np.ascontiguousarray(cw[:, ch].transpose(2, 1, 0)),
        "conv_b": np.ascontiguousarray(W["conv_b"][l][ch].T),
        "gate_b": np.ascontiguousarray(np.stack([W["b_igate"][l][2 * j:2 * j + 2],
                                                 W["b_fgate"][l][2 * j:2 * j + 2]], 1)),
        "g_mout": np.ascontiguousarray(W["g_mlstm_out"][l][2 * j:2 * j + 2].T),
        "g_moba_qk": rep(np.concatenate([gq, gq, gk, gk])),
        "g_moba_out": np.ascontiguousarray(W["g_moba_out"][l][2 * j:2 * j + 2].T),
        "g_cq": np.ascontiguousarray(W["g_cq"][l].reshape(3, 128).T),
        "g_ckv": np.ascontiguousarray(W["g_ckv"][l].reshape(2, 128).T),
        "w_uq": np.ascontiguousarray(W["w_uq"][l][:, 192 * j:192 * j + 192]),
        "w_ukv": np.ascontiguousarray(W["w_ukv"][l][:, 256 * j:256 * j + 256]),
        "g_mla_q": rep(W["g_mla_q"][l]),
        "g_mla_k": rep(W["g_mla_k"][l]),
        "g_mla_out": np.ascontiguousarray(W["g_mla_out"][l][2 * j:2 * j + 2].T),
        "invf": rep(rope_inv_freq()),
    }


def prep_b_inputs(W, l, j, x_b, pos_b):
    S = x_b.shape[0]
    m = prep_b_weights(W, l, j)
    m["x"] = np.ascontiguousarray(x_b)
    m["pos"] = np.ascontiguousarray(pos_b.reshape(S // 128, 128).T.astype(np.int32))
    return m


BIGNEG = 30000.0


def emit_attn(p, S, d, ps, kind):
    nc = p.nc
    moba = kind == "moba"
    Dk = 64 if moba else 96
    scale = SC_MOBA if moba else SC_MLA
    NQG = S // 512
    NKB = S // 128
    NB = S // 256
    psb = [t[:].bitcast(BF16) for t in ps]
    onesf = p.sb([128, 896], F32)
    maskf = p.sb([128, 896], F32)
    mask = p.sb([128, 896], BF16)
    p.op("pool", lambda e: e.memset(onesf[:], 1.0), writes=["a_ones"])
    p.op("pool", lambda e: e.affine_select(out=maskf[:], in_=onesf[:], pattern=[[1, 896]], compare_op=ALU.is_ge,
                                           fill=0.0, base=-384, channel_multiplier=-1),
         reads=["a_ones"], writes=["a_maskf"])
    op_copy(p, "pool", mask[:], maskf[:], ["a_maskf"], ["a_mask"])
    wsel = p.sb([65, 64], F32)
    p.op("pool", lambda e: e.memset(wsel[0:64, :], 1.0 / 64), writes=["a_wsel"])
    p.op("pool", lambda e: e.memset(wsel[64:65, :], EPS), writes=["a_wsel"])
    gout = p.sb([64, 2], F32)
    p.dma("sp", gout[:], d["g_moba_out" if moba else "g_mla_out"], writes=["a_gout"])
    V = p.sb([128, S // 512, 4, 2, 66], BF16)
    p.dma("sp", V[:].rearrange("p t s h w -> p t (s h w)"), d["av" if moba else "lv"].rearrange("t p n -> p t n"),
          writes=["a_V"])
    pT = [p.sb([128, 512], BF16) for _ in range(3)]
    osb = p.sb([65, 512], F32)
    osq = p.sb([65, 512], F32)
    lnn = p.sb([64, 512], F32)
    rsn = p.sb([64, 512], F32)
    mixo = [p.sb([64, 512], BF16) for _ in range(2)]
    if moba:
        kT = p.sb([128, S], BF16)
        qTall = p.sb([128, S], BF16)
        p.dma("sp", kT[:], d["akT"], writes=["a_kT"])
        p.dma("sp", qTall[:], d["aqT"], writes=["a_qT"])
        kmf = p.sb([128, NB], F32)
        kmb = p.sb([128, NB], BF16)
        p.dma("sp", kmf[:], d["kmT"], writes=["a_kmf"])
        op_copy(p, "dve", kmb[:], kmf[:], ["a_kmf"], ["a_kmb"])
        ebf = p.sb([32, 4096], F32)
        ebig = p.sb([32, 4096], BF16)
        p.op("pool", lambda e: e.memset(ebf[:], BIGNEG), writes=["a_ebf"])
        p.op("pool", lambda e: e.affine_select(out=ebf[:], in_=ebf[:], pattern=[[1, 4096]], compare_op=ALU.is_ge,
                                               fill=0.0, base=0, channel_multiplier=-128),
             reads=["a_ebf"], writes=["a_ebf"])
        p.op("pool", lambda e: e.affine_select(out=ebf[:], in_=ebf[:], pattern=[[-1, 4096]], compare_op=ALU.is_ge,
                                               fill=0.0, base=127, channel_multiplier=128),
             reads=["a_ebf"], writes=["a_ebf"])
        op_copy(p, "pool", ebig[:], ebf[:], ["a_ebf"], ["a_ebig"])
        selTs = [p.sb([32, S], BF16) for _ in range(2)]
        gbuf = p.sb([128, 32], F32)
        m8 = p.sb([128, 8], F32)
        thr = p.sb([128, 1], F32)
        selm = p.sb([128, 4, 32], BF16)
        identb = p.sb([128, 128], BF16)
        make_ident(p, identb)
    else:
        kTh = p.sb([96, S], BF16)
        qTt = [p.sb([96, 512], BF16) for _ in range(2)]
    sc = {"s": 0, "o": 0, "p": 0}
    row0 = 256 if moba else 384

    for h in range(2):
        if moba:
            hp = slice(64 * h, 64 * h + 64)
            selT = selTs[h]
            p.op("pool", lambda e: e.memset(gbuf[:], -1e30), reads=[], writes=["a_gbuf"])
            for t in range(S // 128):
                own = t // 2
                if own > 0:
                    p.op("pe", lambda e, t=t, hp=hp: e.matmul(ps[6][:, 0:NB], lhsT=qTall[hp, t * 128:(t + 1) * 128],
                                                              rhs=kmb[hp, 0:NB], start=True, stop=True),
                         reads=["a_qT", "a_kmb"], writes=[("ps", 6)])
                    op_copy(p, "dve", gbuf[:, 0:own], ps[6][:, 0:own], [("ps", 6)], ["a_gbuf"])
                p.op("dve", lambda e: e.max(out=m8[:], in_=gbuf[:]), reads=["a_gbuf"], writes=["a_m8"])
                op_ts(p, "dve", thr[:], m8[:, 2:3], -1e29, ALU.max, ["a_m8"], ["a_thr"])
                op_ts(p, "dve", selm[:, t % 4, :], gbuf[:], thr[:, 0:1], ALU.is_ge, ["a_gbuf", "a_thr"], ["a_selm"],
                      s2=-1.0, op1=ALU.add)
                if t % 4 == 3:
                    def trs(e, selm=selm):
                        for s4 in range(4):
                            r = e.transpose(psb[7][0:32, s4 * 128:(s4 + 1) * 128], selm[:, s4, :], identb[:])
                        return r
                    p.op("pe", trs, reads=["a_selm", "ident2"], writes=[("ps", 7)])
                    op_copy(p, "act", selT[:, (t - 3) * 128:(t + 1) * 128], psb[7][0:32, 0:512], [("ps", 7)], ["a_selT"])
        else:
            p.dma("sp", kTh[:], d["lkT"][h], reads=[], writes=["a_kT"])
        for qg in range(NQG):
            q0 = qg * 512
            if moba:
                qT = qTall[hp, q0:q0 + 512]
                qtok = "a_qT"
            else:
                qb = (h * NQG + qg) % 2
                p.dma("sp", qTt[qb][:], d["lqT"][h][:, q0:q0 + 512], writes=[("a_qTt", qb)])
                qT = qTt[qb][:]
                qtok = ("a_qTt", qb)
            ob = 3 + sc["o"] % 2
            sc["o"] += 1
            nkb = 4 * qg + 4
            for kb in range(nkb):
                r = kb - 4 * qg
                sb_ = sc["s"] % 3
                sc["s"] += 1
                pb = sc["p"] % 3
                sc["p"] += 1
                kTs = (kT[hp, kb * 128:(kb + 1) * 128] if moba else kTh[:, kb * 128:(kb + 1) * 128])
                lo = 256 if (moba and r >= 2) else 0

                def qk(e, kTs=kTs, qT=qT, sb_=sb_, r=r, lo=lo, kb=kb, q0=q0, selT=(selT if moba else None)):
                    bias = moba and r < 2
                    ins = e.matmul(ps[sb_][:, lo:512], lhsT=kTs, rhs=qT[:, lo:512], start=True, stop=not bias)
                    if bias:
                        n = kb // 2
                        c0 = 0 if r < 0 else 256
                        ins = e.matmul(ps[sb_][:, c0:512], lhsT=ebig[0:32, n * 128:(n + 1) * 128],
                                       rhs=selT[0:32, q0 + c0:q0 + 512], start=False, stop=True)
                    return ins
                p.op("pe", qk, reads=["a_kT", qtok] + (["a_ebig", "a_selT"] if moba else []), writes=[("ps", sb_)])
                op_act(p, pT[pb][:, lo:512], ps[sb_][:, lo:512], AF.Exp, [("ps", sb_)], [("a_pT", pb)], scale=scale)
                if r >= 0:
                    if moba:
                        off = (r % 2) * 128
                        mlo = lo
                        mhi = lo + 256
                    else:
                        off = r * 128
                        mlo, mhi = 0, 512
                    op_tt(p, "pool", pT[pb][:, mlo:mhi], pT[pb][:, mlo:mhi], mask[:, 384 - off:384 - off + (mhi - mlo)],
                          ALU.mult, [("a_pT", pb), "a_mask"], [("a_pT", pb)])
                p.op("pe", lambda e, kb=kb, pb=pb, ob=ob, lo=lo, h=h, nkb=nkb: e.matmul(
                    ps[ob][0:65, lo:512], lhsT=V[:, kb // 4, kb % 4, h, 0:65], rhs=pT[pb][:, lo:512],
                    start=(kb == 0), stop=(kb == nkb - 1)),
                    reads=["a_V", ("a_pT", pb)], writes=[("ps", ob)])
            op_copy(p, "act", osb[:], ps[ob][0:65, :], [("ps", ob)], ["a_osb"])
            op_tt(p, "pool", osq[:], osb[:], osb[:], ALU.mult, ["a_osb"], ["a_osq"])
            p.op("pe", lambda e: e.matmul(ps[5][0:64, :], lhsT=wsel[:], rhs=osq[:], start=True, stop=True),
                 reads=["a_wsel", "a_osq"], writes=[("ps", 5)])
            op_act(p, lnn[:], ps[5][0:64, :], AF.Ln, [("ps", 5)], ["a_lnn"])
            op_act(p, rsn[:], lnn[:], AF.Exp, ["a_lnn"], ["a_rsn"], scale=-0.5)
            mb = (h * NQG + qg) % 2
            op_stt(p, mixo[mb][:], osb[0:64, :], gout[:, h:h + 1], rsn[:], ALU.mult, ALU.mult,
                   ["a_osb", "a_gout", "a_rsn"], [("a_mixo", mb)])
            p.dma("sp", d["mixT"][row0 + 64 * h:row0 + 64 * h + 64, q0:q0 + 512], mixo[mb][:],
                  reads=[("a_mixo", mb)], writes=["mixT"])


def emit_mlstm(p, S, d, ps, ident):
    import os
    MST = int(os.environ.get("M_STOP", "99"))
    nc = p.nc
    NC = S // 128
    NTL = S // 512
    psb = [t[:].bitcast(BF16) for t in ps]
    it = p.sb([2, S], F32)
    ft = p.sb([2, S], F32)
    Bn = p.sb([2, S], F32)
    av_ = it
    Mx = ft
    zer1 = p.sb([2, 1], F32)
    zer = zer1[:, 0:1].to_broadcast([2, S])
    Mref = p.sb([2, NC], F32)
    Mprev = p.sb([2, NC], F32)
    gg = p.sb([2, NC], F32)
    i2a = p.sb([2, 2], F32)
    i2 = p.sb([2, 2], F32)
    sel2a = p.sb([2, 128], F32)
    sel2 = p.sb([2, 128], F32)
    ue = p.sb([128, NC, 4], F32)
    gbc = p.sb([128, NC], F32)
    gmo = p.sb([128, 2], F32)
    cm_f = p.sb([128, 128], F32)
    cm_o = p.sb([128, 128], F32)
    cmask = p.sb([128, 128], BF16)
    p.dma("sp", it[:], d["gi"], writes=["m_it"])
    p.dma("sp", ft[:], d["gf"], writes=["m_ft"])
    p.dma("sp", gmo[:], d["g_mout"], writes=["m_gmo"])
    p.op("pool", lambda e: e.memset(zer1[:], 0.0), writes=["m_zer"])
    p.op("pool", lambda e: e.memset(i2a[:], 1.0), writes=["m_i2a"])
    p.op("pool", lambda e: e.affine_select(out=i2[:], in_=i2a[:], pattern=[[1, 2]], compare_op=ALU.is_equal,
                                           fill=0.0, base=0, channel_multiplier=-1), reads=["m_i2a"], writes=["m_i2"])
    p.op("pool", lambda e: e.memset(sel2a[:], 1.0), writes=["m_sel2a"])
    p.op("pool", lambda e: e.affine_select(out=sel2a[:], in_=sel2a[:], pattern=[[1, 128]], compare_op=ALU.is_ge,
                                           fill=0.0, base=0, channel_multiplier=-64), reads=["m_sel2a"], writes=["m_sel2a"])
    p.op("pool", lambda e: e.affine_select(out=sel2[:], in_=sel2a[:], pattern=[[-1, 128]], compare_op=ALU.is_ge,
                                           fill=0.0, base=63, channel_multiplier=64), reads=["m_sel2a"], writes=["m_sel2"])
    p.op("pool", lambda e: e.memset(cm_o[:], 1.0), writes=["m_cmo"])
    p.op("pool", lambda e: e.affine_select(out=cm_f[:], in_=cm_o[:], pattern=[[1, 128]], compare_op=ALU.is_ge,
                                           fill=0.0, base=0, channel_multiplier=-1), reads=["m_cmo"], writes=["m_cmf"])
    op_copy(p, "pool", cmask[:], cm_f[:], ["m_cmf"], ["m_cmask"])
    op_act(p, ft[:], ft[:], AF.Exp, ["m_ft"], ["m_ft"], scale=-1.0)
    op_act(p, ft[:], ft[:], AF.Ln, ["m_ft"], ["m_ft"], bias=1.0)
    p.op("dve", lambda e: e.tensor_tensor_scan(out=Bn[:], data0=ft[:], data1=zer, initial=0.0, op0=ALU.add,
                                               op1=ALU.add), reads=["m_ft", "m_zer"], writes=["m_Bn"])
    op_tt(p, "dve", av_[:], it[:], Bn[:], ALU.add, ["m_it", "m_Bn"], ["m_it", "m_a"])
    p.op("dve", lambda e: e.tensor_tensor_scan(out=Mx[:], data0=av_[:], data1=av_[:], initial=0.0, op0=ALU.max,
                                               op1=ALU.max), reads=["m_a", "m_Bn", "m_ft"], writes=["m_M", "m_ft"])
    op_copy(p, "dve", Mref[:], Mx[:].rearrange("p (c t) -> p c t", t=128)[:, :, 127], ["m_M"], ["m_Mref"])
    p.op("pool", lambda e: e.memset(Mprev[:, 0:1], 0.0), writes=["m_Mprev"])
    if NC > 1:
        op_copy(p, "pool", Mprev[:, 1:NC], Mref[:, 0:NC - 1], ["m_Mref"], ["m_Mprev"])
    op_tt(p, "dve", gg[:], Mprev[:], Mref[:], ALU.subtract, ["m_Mprev", "m_Mref"], ["m_gg"])
    op_act(p, gg[:], gg[:], AF.Exp, ["m_gg"], ["m_gg"])
    mb = Mref[:].unsqueeze(2).to_broadcast([2, NC, 128])
    op_tt(p, "dve", av_[:].rearrange("p (c t) -> p c t", t=128), av_[:].rearrange("p (c t) -> p c t", t=128), mb,
          ALU.subtract, ["m_a", "m_Mref"], ["m_a"])
    op_act(p, av_[:], av_[:], AF.Exp, ["m_a"], ["m_u"])
    op_tt(p, "dve", Bn[:].rearrange("p (c t) -> p c t", t=128), Bn[:].rearrange("p (c t) -> p c t", t=128), mb,
          ALU.subtract, ["m_Bn", "m_Mref"], ["m_Bn"])
    op_act(p, Bn[:], Bn[:], AF.Exp, ["m_Bn"], ["m_e"])

    if MST < 1:
        return

    def tru(e):
        for c in range(NC):
            e.matmul(ps[7][:, c * 4:c * 4 + 2], lhsT=av_[:, c * 128:(c + 1) * 128], rhs=i2[:], start=True, stop=True)
            r = e.matmul(ps[7][:, c * 4 + 2:c * 4 + 4], lhsT=Bn[:, c * 128:(c + 1) * 128], rhs=i2[:], start=True, stop=True)
        return r
    p.op("pe", tru, reads=["m_u", "m_e", "m_i2"], writes=[("ps", 7)])
    op_copy(p, "dve", ue[:].rearrange("p c f -> p (c f)"), ps[7][:, 0:NC * 4], [("ps", 7)], ["m_ue"])
    p.op("pe", lambda e: e.matmul(ps[7][:, 0:NC], lhsT=sel2[:], rhs=gg[:], start=True, stop=True),
         reads=["m_sel2", "m_gg"], writes=[("ps", 7)])
    op_copy(p, "dve", gbc[:], ps[7][:, 0:NC], [("ps", 7)], ["m_gbc"])

    if MST < 2:
        return
    qTt = [p.sb([128, 512], BF16) for _ in range(2)]
    kTt = [p.sb([128, 512], BF16) for _ in range(2)]
    vt = [p.sb([128, 4, 256], BF16) for _ in range(2)]
    ot = [p.sb([128, 4, 256], BF16) for _ in range(2)]
    ktm = p.sb([128, 128], BF16)
    vaug = p.sb([128, 2, 130], BF16)
    Sm = p.sb([128, 2, 128], BF16)
    dmax = p.sb([128, 2], F32)
    rec = p.sb([128, 2], F32)
    ssq = p.sb([128, 2], F32)
    lnq = p.sb([128, 2], F32)
    rsd = p.sb([128, 2], F32)
    scl = p.sb([128, 2], F32)
    junk = p.sb([128, 128], BF16)
    hn = p.sb([128, 2, 128], BF16)
    Cst = p.sb([128, 130], F32)
    Gf = p.sb([128, 130], F32)
    G = p.sb([128, 130], BF16)
    mixm = [p.sb([128, 512], BF16) for _ in range(2)]
    p.op("pool", lambda e: e.memset(Gf[:], 0.0), writes=["m_Gf"])
    p.op("pool", lambda e: e.memset(G[:], 0.0), writes=["m_G"])
    p.op("pool", lambda e: e.memset(Cst[:], 0.0), writes=["m_Cst"])
    na = [0]

    def load(T):
        b = T % 2
        t0 = T * 512
        p.dma("sp", qTt[b][:], d["mqT"][:, t0:t0 + 512], writes=[("m_q", b)])
        p.dma("sp", kTt[b][:], d["mkT"][:, t0:t0 + 512], writes=[("m_k", b)])
        p.dma("sp", vt[b][:].rearrange("p s n -> p (s n)"), d["mv"][T], writes=[("m_v", b)])
        p.dma("sp", ot[b][:].rearrange("p s n -> p (s n)"), d["mo"][T], writes=[("m_o", b)])
    load(0)
    for T in range(NTL):
        b = T % 2
        t0 = T * 512
        if T + 1 < NTL:
            load(T + 1)
        for s in range(4):
            c = T * 4 + s
            cs = slice(s * 128, (s + 1) * 128)
            p.op("pe", lambda e, b=b, cs=cs: e.transpose(psb[0][:, 0:128], kTt[b][:, cs], ident[:]),
                 reads=[("m_k", b), "ident"], writes=[("ps", 0)])
            op_copy(p, "act", ktm[:], psb[0][:, 0:128], [("ps", 0)], ["m_ktm"])
            MSUB = int(os.environ.get("M_SUB", "9"))
            if MSUB < 2:
                continue
            for h in range(2):
                op_ts(p, "dve", vaug[:, h, 0:128], vt[b][:, s, h * 128:(h + 1) * 128],
                      ue[:, c, h:h + 1], ALU.mult, [("m_v", b), "m_ue"], ["m_vaug%d" % h])
            op_copy(p, "dve", vaug[:, :, 128:129], ue[:, c, 0:2].unsqueeze(2), ["m_ue"], ["m_vaug0", "m_vaug1"])

            if MSUB < 3:
                continue

            def st(e, b=b, cs=cs):
                for h in range(2):
                    hp = slice(64 * h, 64 * h + 64)
                    r = e.matmul(ps[1 if h == 0 else 7][:, 0:128], lhsT=kTt[b][hp, cs], rhs=qTt[b][hp, cs],
                                 start=True, stop=True)
                return r
            p.op("pe", st, reads=[("m_k", b), ("m_q", b)], writes=[("ps", 1), ("ps", 7)])
            if MSUB < 4:
                continue
            for h in range(2):
                bk = 1 if h == 0 else 7
                op_tt(p, "dve", Sm[:, h, :], ps[bk][:, 0:128], cmask[:], ALU.mult, [("ps", bk), "m_cmask"], ["m_Sm"])
            if MST < 3:
                continue
            ab = 2 + na[0] % 2
            na[0] += 1
            accv = ps[ab][:, 0:260].rearrange("p (h w) -> p h w", h=2)

            def acc(e, b=b, cs=cs, accv=accv):
                for h in range(2):
                    hp = slice(64 * h, 64 * h + 64)
                    e.matmul(accv[:, h, 0:129], lhsT=Sm[:, h, :], rhs=vaug[:, h, 0:129], start=True, stop=False)
                    r = e.matmul(accv[:, h, 0:129], lhsT=qTt[b][hp, cs], rhs=G[hp, 0:129], start=False, stop=True)
                return r
            p.op("pe", acc, reads=["m_Sm", "m_vaug0", "m_vaug1", ("m_q", b), "m_G"], writes=[("ps", ab)])
            if MST < 4:
                continue
            op_act(p, dmax[:].unsqueeze(2), accv[:, :, 128:129], AF.Abs, [("ps", ab)], ["m_dabs"])
            op_tt(p, "dve", dmax[:], dmax[:], ue[:, c, 2:4], ALU.max, ["m_dabs", "m_ue"], ["m_dmax"])
            p.op("dve", lambda e: e.reciprocal(rec[:], dmax[:]), reads=["m_dmax"], writes=["m_rec"])
            for h in range(2):
                op_act(p, junk[:], accv[:, h, 0:128], AF.Square, [("ps", ab), "m_rec"], ["m_junk", "m_ssq"],
                       scale=rec[:, h:h + 1], accum=ssq[:, h:h + 1])
            op_act(p, lnq[:], ssq[:], AF.Ln, ["m_ssq"], ["m_lnq"], scale=1.0 / 128, bias=EPS)
            op_act(p, rsd[:], lnq[:], AF.Exp, ["m_lnq"], ["m_rsd"], scale=-0.5)
            op_tt(p, "dve", scl[:], rec[:], rsd[:], ALU.mult, ["m_rec", "m_rsd"], ["m_scl"])
            for h in range(2):
                op_stt(p, hn[:, h, :], accv[:, h, 0:128], scl[:, h:h + 1], ot[b][:, s, h * 128:(h + 1) * 128],
                       ALU.mult, ALU.mult, [("ps", ab), "m_scl", ("m_o", b)], ["m_hn"])

            if MST < 5:
                continue

            def tro(e, s=s):
                for h in range(2):
                    r = e.transpose(psb[4 + h][:, s * 128:(s + 1) * 128], hn[:, h, :], ident[:])
                return r
            p.op("pe", tro, reads=["m_hn", "ident"], writes=[("ps", 4), ("ps", 5)])
            if c + 1 < NC and MST >= 6:
                kvv = ps[6][:, 0:260].rearrange("p (h w) -> p h w", h=2)

                def kv(e, kvv=kvv):
                    for h in range(2):
                        r = e.matmul(kvv[:, h, 0:129], lhsT=ktm[:], rhs=vaug[:, h, 0:129], start=True, stop=True)
                    return r
                p.op("pe", kv, reads=["m_ktm", "m_vaug0", "m_vaug1"], writes=[("ps", 6)])
                for h in range(2):
                    hp = slice(64 * h, 64 * h + 64)
                    op_tt(p, "dve", Cst[hp, 0:129], kvv[hp, h, 0:129], Gf[hp, 0:129], ALU.add,
                          [("ps", 6), "m_Gf"], ["m_Cst"])
                op_ts(p, "dve", Gf[:, 0:129], Cst[:, 0:129], gbc[:, c + 1:c + 2], ALU.mult, ["m_Cst", "m_gbc"], ["m_Gf"])
                op_copy(p, "act", G[:, 0:129], Gf[:, 0:129], ["m_Gf"], ["m_G"])
        if MST < 5:
            continue
        for h in range(2):
            mbf = (T * 2 + h) % 2
            op_ts(p, "dve" if h == 0 else "act", mixm[mbf][:], psb[4 + h][:, 0:512], gmo[:, h:h + 1], ALU.mult,
                  [("ps", 4 + h), "m_gmo"], [("m_mix", mbf)]) if h == 0 else \
                op_act(p, mixm[mbf][:], psb[4 + h][:, 0:512], AF.Copy, [("ps", 4 + h), "m_gmo"], [("m_mix", mbf)],
                       scale=gmo[:, h:h + 1])
            p.dma("sp", d["mixT"][h * 128:(h + 1) * 128, t0:t0 + 512], mixm[mbf][:], reads=[("m_mix", mbf)],
                  writes=["mixT"])


_NC_CACHE = {}


def _get_nc(kind):
    if kind not in _NC_CACHE:
        _NC_CACHE[kind] = build_phase_b(SEQ) if kind == "b" else build_phase_c(SEQ // 2)
    return _NC_CACHE[kind]


def assemble_mix(mix_halves):
    S = mix_halves[0].shape[1]
    full = np.empty((1024, S), dtype=mix_halves[0].dtype)
    for j in range(2):
        m = mix_halves[j]
        for hh in range(2):
            hg = 2 * j + hh
            full[hg * 128:(hg + 1) * 128] = m[hh * 128:(hh + 1) * 128]
            full[512 + hg * 64:512 + (hg + 1) * 64] = m[256 + hh * 64:256 + (hh + 1) * 64]
            full[768 + hg * 64:768 + (hg + 1) * 64] = m[384 + hh * 64:384 + (hh + 1) * 64]
    return full


def kernel(**inputs):
    x = np.asarray(inputs["x"], dtype=np.float32)
    pos = np.asarray(inputs["positions"]).astype(np.int32)
    W = {k: np.asarray(v) for k, v in inputs.items() if k not in ("x", "positions")}
    half = SEQ // 2
    cur = x
    for l in range(DEPTH):
        wj = [prep_b_weights(W, l, j) for j in range(2)]
        in_maps = []
        for c in range(N_CORES):
            b, j = c // 2, c % 2
            m = dict(wj[j])
            m["x"] = np.ascontiguousarray(cur[b])
            m["pos"] = np.ascontiguousarray(pos[b].reshape(SEQ // 128, 128).T)
            in_maps.append(m)
        res = run_bass_kernel_spmd(build_phase_b(SEQ), in_maps, core_ids=list(range(N_CORES)))
        mixes = [assemble_mix([np.asarray(res.results[2 * b + j]["mixT"]) for j in range(2)]) for b in range(BATCH)]
        in_maps = []
        g_mlp = np.ascontiguousarray(W["g_mlp_norm"][l].reshape(8, 128).T)
        for c in range(N_CORES):
            b, hf = c // 2, c % 2
            in_maps.append({
                "x_in": np.ascontiguousarray(cur[b, hf * half:(hf + 1) * half]),
                "mixT": np.ascontiguousarray(mixes[b][:, hf * half:(hf + 1) * half]),
                "w_out": np.ascontiguousarray(W["w_out"][l]),
                "w_up": np.ascontiguousarray(W["w_up"][l]),
                "w_down": np.ascontiguousarray(W["w_down"][l]),
                "g_mlp": g_mlp,
            })
        res = run_bass_kernel_spmd(build_phase_c(half), in_maps, core_ids=list(range(N_CORES)))
        nxt = np.empty_like(cur)
        for c in range(N_CORES):
            b, hf = c // 2, c % 2
            nxt[b, hf * half:(hf + 1) * half] = np.asarray(res.results[c]["x_out"])
        cur = nxt
    return cur.astype(np.float32)
```
